# Optimizing a Trainium2 kernel written in Bass

```python
import math
import jax, jax.numpy as jnp
from jax import lax
import numpy as np

D_MODEL = 1024
BATCH = 8
SEQ = 4096
DEPTH = 2

CHUNK = 64
EPS = 1e-6
A_HEADS = 8
A_HEAD_DIM = 64
A_WIDTH = A_HEADS * A_HEAD_DIM
KV_RANK = 128
IDX_HEADS = 4
IDX_DIM = 64
TOPK_MAX = 256
Q_BLOCK = 128
S5_WIDTH = 512
S5_GROUP = 16
S5_GROUPS = S5_WIDTH // S5_GROUP
S5_STATE = 64
DT_MIN = 0.001
DT_MAX = 0.1
C_HEADS = 4
C_HEAD_DIM = 128
C_WIDTH = C_HEADS * C_HEAD_DIM
C_CONV = 4
D_FF = 2816
FFN_CONV = 3
N_BRANCH = 3
IN_SPLITS = (A_WIDTH, KV_RANK, IDX_HEADS * IDX_DIM, IDX_DIM, IDX_HEADS,
             S5_WIDTH,
             3 * C_WIDTH, C_WIDTH, C_HEADS, C_HEADS,
             N_BRANCH * D_MODEL)
N_IN = (A_WIDTH + KV_RANK + IDX_HEADS * IDX_DIM + IDX_DIM + IDX_HEADS + S5_WIDTH
        + 3 * C_WIDTH + C_WIDTH + 2 * C_HEADS + N_BRANCH * D_MODEL)

kernel_name = 'hybrid_dsa_s5_gdn_encoder'


def rms_norm(x, g):
    xf = x.astype(jnp.float32)
    y = xf * lax.rsqrt(jnp.mean(xf * xf, axis=-1, keepdims=True) + EPS)
    return (y * g.astype(jnp.float32)).astype(x.dtype)


def l2_norm(x):
    return x * lax.rsqrt(jnp.sum(x * x, axis=-1, keepdims=True) + EPS)


def causal_dwconv(x, w):
    k = w.shape[0]
    t = x.shape[1]
    xp = jnp.pad(x, ((0, 0), (k - 1, 0), (0, 0)))
    out = xp[:, 0:t] * w[0]
    for j in range(1, k):
        out = out + xp[:, j:j + t] * w[j]
    return out


def dsa_attention(q, c_kv, iq, ik, iw, kv_norm_g, w_uk, w_uv):
    b, t = q.shape[0], q.shape[1]
    topk = min(TOPK_MAX, t // 4)
    nb = t // Q_BLOCK
    c_kv = rms_norm(c_kv, kv_norm_g)
    q_lat = jnp.einsum('bthd,rhd->bthr', q, w_uk) * (A_HEAD_DIM ** -0.5)
    iw = iw * (IDX_HEADS ** -0.5)
    key_pos = jnp.arange(t)

    def to_blocks(a):
        a = a.reshape((b, nb, Q_BLOCK) + a.shape[2:])
        return jnp.moveaxis(a, 1, 0)

    def block(args):
        qb, iqb, iwb, pos = args
        limit = (pos // CHUNK + 1) * CHUNK
        adm = key_pos[None, :] < limit[:, None]
        rel = jax.nn.relu(jnp.einsum('bqhd,bsd->bqhs', iqb, ik) * (IDX_DIM ** -0.5))
        score = jnp.einsum('bqhs,bqh->bqs', rel, iwb).astype(jnp.float32)
        score = jnp.where(adm[None], score, -jnp.inf)
        _, idx = lax.top_k(score, topk)
        valid = idx < limit[None, :, None]
        c_sel = jax.vmap(lambda c, i: c[i])(c_kv, idx)
        logits = jnp.einsum('bqhr,bqkr->bqhk', qb, c_sel).astype(jnp.float32)
        logits = jnp.where(valid[:, :, None, :], logits, -jnp.inf)
        p = jax.nn.softmax(logits, axis=-1).astype(c_sel.dtype)
        o_lat = jnp.einsum('bqhk,bqkr->bqhr', p, c_sel)
        return jnp.einsum('bqhr,rhd->bqhd', o_lat, w_uv)

    pos_blocks = key_pos.reshape(nb, Q_BLOCK)
    out = lax.map(block, (to_blocks(q_lat), to_blocks(iq), to_blocks(iw), pos_blocks))
    return jnp.moveaxis(out, 0, 1).reshape(b, t, A_WIDTH)


def _complex_affine_combine(e1, e2):
    a1r, a1i, b1r, b1i = e1
    a2r, a2i, b2r, b2i = e2
    return (a1r * a2r - a1i * a2i,
            a1r * a2i + a1i * a2r,
            a2r * b1r - a2i * b1i + b2r,
            a2r * b1i + a2i * b1r + b2i)


def s5_branch(u, a_re, a_im, log_dt, b_re, b_im, c_re, c_im, d, w_glu):
    bsz, t = u.shape[0], u.shape[1]
    f32 = jnp.float32
    uf = u.astype(f32).reshape(bsz, t, S5_GROUPS, S5_GROUP)
    a_re, a_im = a_re.astype(f32), a_im.astype(f32)
    b_re, b_im = b_re.astype(f32), b_im.astype(f32)
    c_re, c_im = c_re.astype(f32), c_im.astype(f32)
    dt = jnp.exp(log_dt.astype(f32))[:, None]
    mag = jnp.exp(dt * a_re)
    ang = dt * a_im
    ab_re = mag * jnp.cos(ang)
    ab_im = mag * jnp.sin(ang)
    den = a_re * a_re + a_im * a_im
    n_re = ab_re - 1.0
    f_re = (n_re * a_re + ab_im * a_im) / den
    f_im = (ab_im * a_re - n_re * a_im) / den
    bb_re = f_re[..., None] * b_re - f_im[..., None] * b_im
    bb_im = f_re[..., None] * b_im + f_im[..., None] * b_re
    x_re = jnp.einsum('btgm,gpm->btgp', uf, bb_re)
    x_im = jnp.einsum('btgm,gpm->btgp', uf, bb_im)
    a_shape = (1, t) + ab_re.shape
    elems = (jnp.broadcast_to(ab_re, a_shape), jnp.broadcast_to(ab_im, a_shape), x_re, x_im)
    _, _, h_re, h_im = lax.associative_scan(_complex_affine_combine, elems, axis=1)
    y = (jnp.einsum('btgp,gmp->btgm', h_re, c_re) - jnp.einsum('btgp,gmp->btgm', h_im, c_im)
         + d.astype(f32).reshape(S5_GROUPS, S5_GROUP) * uf)
    y = jax.nn.gelu(y.reshape(bsz, t, S5_WIDTH).astype(u.dtype))
    return y * jax.nn.sigmoid(y @ w_glu)


def gated_delta_rule(q, k, v, g, beta):
    bsz, t, h, dk = q.shape
    dv = v.shape[-1]
    n = t // CHUNK

    def chunks(a):
        a = jnp.moveaxis(a, 2, 1)
        return a.reshape((bsz, h, n, CHUNK) + a.shape[3:])

    q = chunks(q) * (dk ** -0.5)
    k, v, g, beta = chunks(k), chunks(v), chunks(g), chunks(beta)
    gam = jnp.cumsum(g, axis=-1)
    i = jnp.arange(CHUNK)
    lower_incl = i[:, None] >= i[None, :]
    strict = i[:, None] > i[None, :]
    decay = jnp.exp(jnp.where(lower_incl, gam[..., :, None] - gam[..., None, :], -jnp.inf))
    kb = k * beta[..., None]
    m = jnp.where(strict, jnp.einsum('bhncd,bhnsd->bhncs', kb, k) * decay, 0.0)
    eye = jnp.eye(CHUNK, dtype=m.dtype)
    rhs = jnp.concatenate([v * beta[..., None], kb * jnp.exp(gam)[..., None]], axis=-1)
    sol = lax.linalg.triangular_solve(m + eye, rhs, left_side=True, lower=True, unit_diagonal=True)
    u, w = sol[..., :dv], sol[..., dv:]
    attn = jnp.einsum('bhncd,bhnsd->bhncs', q, k) * decay
    q_dec = q * jnp.exp(gam)[..., None]
    k_dec = k * jnp.exp(gam[..., -1:] - gam)[..., None]
    last = jnp.exp(gam[..., -1])

    def step(s, xs):
        u_c, w_c, a_c, qd_c, kd_c, l_c = xs
        v_new = u_c - jnp.einsum('bhcd,bhde->bhce', w_c, s)
        o = jnp.einsum('bhcd,bhde->bhce', qd_c, s) + jnp.einsum('bhcs,bhse->bhce', a_c, v_new)
        s = s * l_c[..., None, None] + jnp.einsum('bhcd,bhce->bhde', kd_c, v_new)
        return s, o

    xs = tuple(jnp.moveaxis(a, 2, 0) for a in (u, w, attn, q_dec, k_dec, last))
    s0 = jnp.zeros((bsz, h, dk, dv), q.dtype)
    _, o = lax.scan(step, s0, xs)
    o = jnp.moveaxis(o, 0, 2).reshape(bsz, h, t, dv)
    return jnp.moveaxis(o, 1, 2)


def gdn_branch(qkv, z, a, bgate, conv_w, a_log, dt_bias, norm_g):
    bsz, t = qkv.shape[0], qkv.shape[1]
    dtype = qkv.dtype
    f32 = jnp.float32
    qkv = jax.nn.silu(causal_dwconv(qkv, conv_w)).astype(f32)
    q, k, v = jnp.split(qkv, 3, axis=-1)
    shp = (bsz, t, C_HEADS, C_HEAD_DIM)
    q = l2_norm(q.reshape(shp))
    k = l2_norm(k.reshape(shp))
    v = v.reshape(shp)
    beta = jax.nn.sigmoid(bgate.astype(f32))
    g = -jnp.exp(a_log.astype(f32)) * jax.nn.softplus(a.astype(f32) + dt_bias.astype(f32))
    o = gated_delta_rule(q, k, v, g, beta)
    o = rms_norm(o, norm_g) * jax.nn.silu(z.astype(f32).reshape(shp))
    return o.reshape(bsz, t, C_WIDTH).astype(dtype)


def conv_glu_ffn(h, w_up, conv_w, w_down):
    hid = causal_dwconv(h @ w_up, conv_w)
    gate, val = jnp.split(hid, 2, axis=-1)
    return (jax.nn.silu(gate) * val) @ w_down


def setup_inputs(seed: int = 0) -> dict:
    key = jax.random.key(seed)
    ks = iter(jax.random.split(key, 32))
    f32 = jnp.float32

    def nrm(shape, scale):
        return jax.random.normal(next(ks), shape, f32) * scale

    L, D = DEPTH, D_MODEL
    G, P, M = S5_GROUPS, S5_STATE, S5_GROUP
    x = nrm((BATCH, SEQ, D), 1.0)
    attn_norm_g = 1.0 + nrm((L, D), 0.01)
    w_in = nrm((L, D, N_IN), D ** -0.5)
    kv_norm_g = 1.0 + nrm((L, KV_RANK), 0.01)
    w_uk = nrm((L, KV_RANK, A_HEADS, A_HEAD_DIM), KV_RANK ** -0.5)
    w_uv = nrm((L, KV_RANK, A_HEADS, A_HEAD_DIM), KV_RANK ** -0.5)
    w_proj_a = nrm((L, A_WIDTH, D), A_WIDTH ** -0.5)
    s5_a_re = -0.5 + nrm((L, G, P), 0.01)
    s5_a_im = math.pi * jnp.arange(P, dtype=f32) + nrm((L, G, P), 0.01)
    s5_log_dt = jax.random.uniform(next(ks), (L, G), f32, math.log(DT_MIN), math.log(DT_MAX))
    s5_b_re = nrm((L, G, P, M), (2 * M) ** -0.5)
    s5_b_im = nrm((L, G, P, M), (2 * M) ** -0.5)
    s5_c_re = nrm((L, G, M, P), 0.5)
    s5_c_im = nrm((L, G, M, P), 0.5)
    s5_d = nrm((L, S5_WIDTH), 1.0)
    w_glu = nrm((L, S5_WIDTH, S5_WIDTH), S5_WIDTH ** -0.5)
    w_proj_b = nrm((L, S5_WIDTH, D), S5_WIDTH ** -0.5)
    gdn_conv_w = nrm((L, C_CONV, 3 * C_WIDTH), C_CONV ** -0.5)
    gdn_a_log = jnp.log(jax.random.uniform(next(ks), (L, C_HEADS), f32, 1.0, 16.0))
    dt = jnp.exp(jax.random.uniform(next(ks), (L, C_HEADS), f32, math.log(DT_MIN), math.log(DT_MAX)))
    gdn_dt_bias = dt + jnp.log(-jnp.expm1(-dt))
    gdn_norm_g = 1.0 + nrm((L, C_HEAD_DIM), 0.01)
    w_proj_c = nrm((L, C_WIDTH, D), C_WIDTH ** -0.5)
    w_out = nrm((L, D, D), D ** -0.5)
    ffn_norm_g = 1.0 + nrm((L, D), 0.01)
    w_up = nrm((L, D, 2 * D_FF), D ** -0.5)
    ffn_conv_w = nrm((L, FFN_CONV, 2 * D_FF), FFN_CONV ** -0.5)
    w_down = nrm((L, D_FF, D), D_FF ** -0.5)
    final_norm_g = 1.0 + nrm((D,), 0.01)
    return {'x': x, 'attn_norm_g': attn_norm_g, 'w_in': w_in,
            'kv_norm_g': kv_norm_g, 'w_uk': w_uk, 'w_uv': w_uv, 'w_proj_a': w_proj_a,
            's5_a_re': s5_a_re, 's5_a_im': s5_a_im, 's5_log_dt': s5_log_dt,
            's5_b_re': s5_b_re, 's5_b_im': s5_b_im, 's5_c_re': s5_c_re, 's5_c_im': s5_c_im,
            's5_d': s5_d, 'w_glu': w_glu, 'w_proj_b': w_proj_b,
            'gdn_conv_w': gdn_conv_w, 'gdn_a_log': gdn_a_log, 'gdn_dt_bias': gdn_dt_bias,
            'gdn_norm_g': gdn_norm_g, 'w_proj_c': w_proj_c,
            'w_out': w_out, 'ffn_norm_g': ffn_norm_g, 'w_up': w_up, 'ffn_conv_w': ffn_conv_w,
            'w_down': w_down, 'final_norm_g': final_norm_g}


def reference(x, attn_norm_g, w_in, kv_norm_g, w_uk, w_uv, w_proj_a,
              s5_a_re, s5_a_im, s5_log_dt, s5_b_re, s5_b_im, s5_c_re, s5_c_im, s5_d, w_glu, w_proj_b,
              gdn_conv_w, gdn_a_log, gdn_dt_bias, gdn_norm_g, w_proj_c,
              w_out, ffn_norm_g, w_up, ffn_conv_w, w_down, final_norm_g):
    bsz, t = x.shape[0], x.shape[1]
    split_points = [int(s) for s in np.cumsum(IN_SPLITS)[:-1]]
    for l in range(DEPTH):
        h = rms_norm(x, attn_norm_g[l])
        (a_q, a_ckv, a_iq, a_ik, a_iw, s5_u, c_qkv, c_z, c_a, c_b,
         gates) = jnp.split(h @ w_in[l], split_points, axis=-1)
        y_a = dsa_attention(a_q.reshape(bsz, t, A_HEADS, A_HEAD_DIM), a_ckv,
                            a_iq.reshape(bsz, t, IDX_HEADS, IDX_DIM), a_ik, a_iw,
                            kv_norm_g[l], w_uk[l], w_uv[l]) @ w_proj_a[l]
        y_b = s5_branch(s5_u, s5_a_re[l], s5_a_im[l], s5_log_dt[l], s5_b_re[l], s5_b_im[l],
                        s5_c_re[l], s5_c_im[l], s5_d[l], w_glu[l]) @ w_proj_b[l]
        y_c = gdn_branch(c_qkv, c_z, c_a, c_b, gdn_conv_w[l], gdn_a_log[l], gdn_dt_bias[l],
                         gdn_norm_g[l]) @ w_proj_c[l]
        g_a, g_b, g_c = jnp.split(jax.nn.sigmoid(gates), N_BRANCH, axis=-1)
        x = x + (g_a * y_a + g_b * y_b + g_c * y_c) @ w_out[l]
        x = x + conv_glu_ffn(rms_norm(x, ffn_norm_g[l]), w_up[l], ffn_conv_w[l], w_down[l])
    return rms_norm(x, final_norm_g)
```

```python
import numpy as np
import concourse.bass as bass
import concourse.mybir as mybir
from concourse.bass_utils import run_bass_kernel_spmd

F32 = mybir.dt.float32
BF16 = mybir.dt.bfloat16
AF = mybir.ActivationFunctionType
ALU = mybir.AluOpType
AX = mybir.AxisListType

ENGS = ("tensor", "vector", "scalar", "gpsimd", "sync")
ROT = 20000
T = 4096
D = 1024
TT = 512
NT = T // TT
DFF = 2816
NEG = -1.0e30
NIT = 26


class Buf:
    __slots__ = ("name", "w", "r", "dsem", "dcnt", "psum")

    def __init__(self, name, w=None):
        self.psum = False
        self.name = name
        self.w = w
        self.r = []
        self.dsem = None
        self.dcnt = 0


class Op:
    __slots__ = ("eng", "fn", "waits", "target", "dma", "tidx")

    def __init__(self, eng, fn):
        self.eng = eng
        self.fn = fn
        self.waits = []
        self.target = False
        self.dma = None
        self.tidx = 0


class Sched:
    def __init__(self, nc):
        self.nc = nc
        self.ops = {e: [] for e in ENGS}
        self.bufs = []
        self.bar_tok = None
        self.dsems = {}

    def buf(self, name):
        b = Buf(name, self.bar_tok)
        self.bufs.append(b)
        return b

    def _dep(self, op, tok):
        if tok is None:
            return
        if tok[0] == "E" and tok[1] == "tensor" and op.eng == "tensor":
            return
        op.waits.append(tok)

    def op(self, eng, fn, r=(), w=()):
        o = Op(eng, fn)
        self.ops[eng].append(o)
        tok = ("E", eng, o)
        for t in r:
            self._dep(o, t.b.w)
            if t.b.psum:
                for x in t.b.r:
                    if x[0] == "E" and x[1] == eng:
                        continue
                    self._dep(o, x)
        for t in w:
            self._dep(o, t.b.w)
            for x in t.b.r:
                if x[0] == "E" and x[1] == eng:
                    continue
                self._dep(o, x)
        for t in r:
            t.b.r.append(tok)
        for t in w:
            t.b.w = tok
            t.b.r = []
        return o

    def dma(self, q, out_ap, in_ap, r=(), w=()):
        dst = w[0].b
        if dst.name not in self.dsems:
            self.dsems[dst.name] = [self.nc.alloc_semaphore("d_" + dst.name), 0]
        ent = self.dsems[dst.name]
        o = Op(q, lambda e: e.dma_start(out=out_ap, in_=in_ap))
        self.ops[q].append(o)
        for t in r:
            self._dep(o, t.b.w)
        for t in w:
            self._dep(o, t.b.w)
            for x in t.b.r:
                self._dep(o, x)
        ent[1] += 16
        o.dma = (ent[0], ent[1])
        tok = ("D", ent[0], ent[1])
        for t in r:
            t.b.r.append(tok)
        for t in w:
            t.b.w = tok
            t.b.r = []
        return o

    def barrier(self, fn, exclude=()):
        ex = set(id(b) for b in exclude)
        o = Op("vector", fn)
        self.ops["vector"].append(o)
        tok = ("E", "vector", o)
        for b in self.bufs:
            if id(b) in ex:
                continue
            self._dep(o, b.w)
            for x in b.r:
                if x[0] == "E" and x[1] == "vector":
                    continue
                self._dep(o, x)
            b.w = tok
            b.r = []
        self.bar_tok = tok

    def emit(self, final_bufs=()):
        nc = self.nc
        for e in ENGS:
            for o in self.ops[e]:
                for t in o.waits:
                    if t[0] == "E":
                        t[2].target = True
        sems = {}
        for e in ENGS:
            n = 0
            for o in self.ops[e]:
                if o.target:
                    n += 1
                    o.tidx = n
            nrot = (n + ROT - 1) // ROT
            sems[e] = [nc.alloc_semaphore(f"s_{e}_{i}") for i in range(nrot)]
        finals = [tuple(self.dsems[n]) for n in final_bufs if n in self.dsems]

        def run(e, engobj):
            seenE = {}
            seenD = {}
            for o in self.ops[e]:
                for t in o.waits:
                    if t[0] == "E":
                        _, eng2, o2 = t
                        if seenE.get(eng2, 0) >= o2.tidx:
                            continue
                        seenE[eng2] = o2.tidx
                        ti = o2.tidx - 1
                        engobj.wait_ge(sems[eng2][ti // ROT], ti % ROT + 1)
                    else:
                        _, sem, val = t
                        if seenD.get(id(sem), 0) >= val:
                            continue
                        seenD[id(sem)] = val
                        engobj.wait_ge(sem, val)
                ins = o.fn(engobj)
                if o.dma is not None:
                    ins.then_inc(o.dma[0], 16)
                elif o.target:
                    ti = o.tidx - 1
                    ins.then_inc(sems[e][ti // ROT], 1)
            if e == "sync":
                for sem, val in finals:
                    engobj.wait_ge(sem, val)

        with nc.Block() as block:
            @block.tensor
            def _(eng):
                run("tensor", eng)

            @block.vector
            def _(eng):
                run("vector", eng)

            @block.scalar
            def _(eng):
                run("scalar", eng)

            @block.gpsimd
            def _(eng):
                run("gpsimd", eng)

            @block.sync
            def _(eng):
                run("sync", eng)


class Tl:
    def __init__(self, S, ap, name, b=None):
        self.ap = ap
        self.b = b if b is not None else S.buf(name)

    def __getitem__(self, idx):
        return self.ap[idx]


C_ID, C_ONE, C_TRIU, C_MINC, C_MSTR, C_TAU, C_RM, C_W = 0, 128, 256, 384, 512, 640, 768, 772


def make_consts():
    c = np.zeros((128, C_W), np.float32)
    i = np.arange(128)
    c[:, C_ID:C_ID + 128] = np.eye(128)
    c[:, C_ONE:C_ONE + 128] = 1.0
    c[:, C_TRIU:C_TRIU + 128] = (i[:, None] <= i[None, :])
    c[:, C_MINC:C_MINC + 128] = (i[None, :] >= i[:, None])
    c[:, C_MSTR:C_MSTR + 128] = (i[:, None] > i[None, :])
    c[:, C_TAU:C_TAU + 128] = i[None, :].astype(np.float32)
    c[:, C_RM:C_RM + 4] = (i[:, None] // 32 == np.arange(4)[None, :])
    return c


SM_GA, SM_GF, SM_KVG, SM_GNG, SM_S5D, SM_GCW, SM_FCW, SM_ALOG, SM_DTB = 0, 8, 16, 17, 18, 22, 70, 202, 206
SM_ARE, SM_AIM, SM_LDT, SM_BRE, SM_BIM, SM_CRE, SM_CIM = 210, 226, 242, 258, 514, 770, 1026
SM_W = 1282


def pack_small(p, l):
    s = np.zeros((128, SM_W), np.float32)
    s[:, SM_GA:SM_GA + 8] = p["attn_norm_g"][l].reshape(8, 128).T
    s[:, SM_GF:SM_GF + 8] = p["ffn_norm_g"][l].reshape(8, 128).T
    s[:, SM_KVG] = p["kv_norm_g"][l]
    s[:, SM_GNG] = p["gdn_norm_g"][l]
    s[:, SM_S5D:SM_S5D + 4] = p["s5_d"][l].reshape(4, 128).T
    s[:, SM_GCW:SM_GCW + 48] = p["gdn_conv_w"][l].reshape(4, 12, 128).transpose(2, 1, 0).reshape(128, 48)
    s[:, SM_FCW:SM_FCW + 132] = p["ffn_conv_w"][l].reshape(3, 44, 128).transpose(2, 1, 0).reshape(128, 132)
    s[:, SM_ALOG:SM_ALOG + 4] = p["gdn_a_log"][l][None, :]
    s[:, SM_DTB:SM_DTB + 4] = p["gdn_dt_bias"][l][None, :]
    def sm(a):
        return a.reshape(16, 2, 64).transpose(1, 2, 0).reshape(128, 16)
    s[:, SM_ARE:SM_ARE + 16] = sm(p["s5_a_re"][l])
    s[:, SM_AIM:SM_AIM + 16] = sm(p["s5_a_im"][l])
    s[:, SM_LDT:SM_LDT + 16] = sm(np.repeat(p["s5_log_dt"][l][:, None], 64, axis=1))
    def smb(a):
        return a.reshape(16, 2, 64, 16).transpose(1, 2, 0, 3).reshape(128, 256)
    s[:, SM_BRE:SM_BRE + 256] = smb(p["s5_b_re"][l])
    s[:, SM_BIM:SM_BIM + 256] = smb(p["s5_b_im"][l])
    s[:, SM_CRE:SM_CRE + 256] = smb(p["s5_c_re"][l].transpose(0, 2, 1))
    s[:, SM_CIM:SM_CIM + 256] = smb(p["s5_c_im"][l].transpose(0, 2, 1))
    return s


W_SPLITS = dict(q=(0, 512), ckv=(512, 640), iq=(640, 896), ik=(896, 960), iw=(960, 964), u=(964, 1476),
                qkv=(1476, 3012), z=(3012, 3524), a=(3524, 3528), b=(3528, 3532), gates=(3532, 6604))


class Builder:
    def __init__(self, n_layers=2, n_tiles=NT, branches=("a", "b", "c"), ffn=True, dbg=None):
        self.L = n_layers
        self.ntiles = n_tiles
        self.branches = branches
        self.ffn = ffn
        self.dbg = dbg or {}
        nc = self.nc = bass.Bass("TRN2", target_bir_lowering=False)
        S = self.S = Sched(nc)
        L = 2

        def din(name, shape):
            return nc.dram_tensor(name, list(shape), F32, kind="ExternalInput").ap()

        self.xT = Tl(S, din("xT", [D, T]), "xT")
        self.d_cst = din("cst", [128, C_W])
        self.d_small = din("small", [L, 128, SM_W])
        self.d_gfin = din("gfin", [128, 8])
        self.dw = {}
        for nm, shp in [("w_q", [L, D, 512]), ("w_ckv", [L, D, 128]), ("w_iq", [L, D, 256]), ("w_ik2", [L, D, 128]),
                        ("w_sm", [L, D, 12]), ("w_u", [L, D, 512]), ("w_qkv", [L, D, 1536]), ("w_z", [L, D, 512]),
                        ("w_g", [L, D, 3072]), ("wuk2", [L, 128, 512]), ("wuvp", [L, 128, 1024]),
                        ("w_pa", [L, 512, D]), ("w_pb", [L, 512, D]), ("w_pc", [L, 512, D]), ("w_glu", [L, 512, 512]),
                        ("w_out", [L, D, D]), ("w_up", [L, D, 2 * DFF]), ("w_down", [L, DFF, D])]:
            self.dw[nm] = din(nm, shp)
        self.x1T = Tl(S, nc.dram_tensor("x1T", [D, T], F32, kind="Internal").ap(), "x1T")
        self.outT = Tl(S, nc.dram_tensor("outT", [D, T], F32, kind="ExternalOutput").ap(), "outT")
        self.dbg_out = {}
        for nm, shp in self.dbg.items():
            self.dbg_out[nm] = Tl(S, nc.dram_tensor(nm, list(shp), F32, kind="ExternalOutput").ap(), nm)

        def sb(name, shape, dt=F32):
            return Tl(S, nc.alloc_sbuf_tensor("sb_" + name, list(shape), dt)[:], name)

        self.cst = sb("cst", [128, C_W])
        self.small = sb("small", [128, SM_W])
        self.gfin = sb("gfin", [128, 8])
        self.slabs = [sb(f"slab{i}", [128, 2048]) for i in range(4)]
        self.slab_i = 0
        self.cT = sb("cT", [128, T])
        self.ctok = sb("ctok", [128, T // 128, 128])
        self.ikT = sb("ikT", [128, T])
        self.gS = sb("gS", [128, 4, 128])
        self.gtail = sb("gtail", [128, 12, 3])
        self.ftail = sb("ftail", [128, 44, 2])
        self.s5c = sb("s5c", [128, 4, 16])
        self.s5k = sb("s5k", [128, 8, 16])
        self.Bc = sb("Bc", [128, 2, 4, 128])
        self.Cz = sb("Cz", [128, 2, 16, 32])
        self.nA = sb("nA", [128, 4])
        self.bar = sb("bar", [128, 1])
        self.arena_h = nc.alloc_sbuf_tensor("arena", [128, ARENA], F32)
        self.ps = [Tl(S, nc.alloc_psum_tensor(f"ps{i}", [128, 512], F32)[:], f"ps{i}") for i in range(8)]
        for p_ in self.ps:
            p_.b.psum = True
        self.aoff = 0

    def av(self, name, shape, dt=F32):
        n = int(np.prod(shape[1:]))
        assert self.aoff + n <= ARENA, (name, self.aoff, n)
        ap = self.arena_h[:, self.aoff:self.aoff + n]
        if len(shape) == 3:
            ap = ap.rearrange("p (a b) -> p a b", a=shape[1])
        elif len(shape) == 4:
            ap = ap.rearrange("p (a b c) -> p a b c", a=shape[1], b=shape[2])
        self.aoff += n
        return Tl(self.S, ap, name)

    def phase(self, keep):
        bar = self.bar
        self.S.barrier(lambda e: e.memset(bar[:], 0.0), exclude=[s.b for s in self.slabs])
        self.aoff = keep

    def mm(self, out, lhsT, rhs, start, stop, r, w, **kw):
        self.S.op("tensor", lambda e: e.matmul(out, lhsT=lhsT, rhs=rhs, start=start, stop=stop, **kw), r=r, w=w)

    def tr(self, out, in_, r, w):
        ident = self.cst[:, C_ID:C_ID + 128]
        self.S.op("tensor", lambda e: e.transpose(out, in_, ident), r=list(r) + [self.cst], w=w)

    def act(self, out, in_, func, r, w, **kw):
        self.S.op("scalar", lambda e: e.activation(out=out, in_=in_, func=func, **kw), r=r, w=w)

    def tt(self, eng, out, in0, in1, op, r, w):
        self.S.op(eng, lambda e: e.tensor_tensor(out=out, in0=in0, in1=in1, op=op), r=r, w=w)

    def ts(self, eng, out, in0, s1, s2, op0, op1, r, w, **kw):
        if op1 is None:
            self.S.op(eng, lambda e: e.tensor_scalar(out=out, in0=in0, scalar1=s1, scalar2=None, op0=op0, **kw), r=r, w=w)
        else:
            self.S.op(eng, lambda e: e.tensor_scalar(out=out, in0=in0, scalar1=s1, scalar2=s2, op0=op0, op1=op1, **kw), r=r, w=w)

    def stt(self, out, in0, sc, in1, op0, op1, r, w):
        self.S.op("vector", lambda e: e.scalar_tensor_tensor(out=out, in0=in0, scalar=sc, in1=in1, op0=op0, op1=op1), r=r, w=w)

    def cp(self, eng, out, in_, r, w):
        if eng == "scalar":
            self.S.op("scalar", lambda e: e.copy(out=out, in_=in_), r=r, w=w)
        else:
            self.S.op(eng, lambda e: e.tensor_copy(out=out, in_=in_), r=r, w=w)

    def memset(self, eng, ap, val, w):
        self.S.op(eng, lambda e: e.memset(ap, val), w=w)

    def slab(self, dram_ap, shape):
        s = self.slabs[self.slab_i]
        self.slab_i = (self.slab_i + 1) % len(self.slabs)
        n = int(np.prod(shape[1:]))
        assert n <= 2048
        ap = s.ap[:, 0:n]
        if len(shape) == 3:
            ap = ap.rearrange("p (a b) -> p a b", a=shape[1])
        self.S.dma("sync", ap, dram_ap, w=[s])
        return Tl(self.S, ap, s.b.name, b=s.b)

    def wslab(self, w_ap, c0, nc_, nk=None, k0=0):
        K = w_ap.shape[0]
        if nk is None:
            nk = K // 128
        v = w_ap.rearrange("(k p) c -> p k c", p=128)[:, k0:k0 + nk, c0:c0 + nc_]
        return self.slab(v, [128, nk, nc_])

    def proj(self, ps, M, w_ap, c0, src, wr, n=TT):
        nk = w_ap.shape[0] // 128
        sl = self.wslab(w_ap, c0, M)
        for k in range(nk):
            self.mm(ps[0:M, 0:n], sl[:, k, :], src[:, k, 0:n], k == 0, k == nk - 1, r=[sl, src], w=[wr])

    def rmsnorm(self, src, g_ap, g_t, dst, tmp, nk=8, n=TT, dim=D):
        ps = self.ps[0]
        ones = self.cst[:, C_ONE:C_ONE + 128]
        for k in range(nk):
            self.act(tmp[:, k, :], src[:, k, :], AF.Square, r=[src], w=[tmp])
        for k in range(nk):
            self.mm(ps[:, 0:n], ones, tmp[:, k, :], k == 0, k == nk - 1, r=[self.cst, tmp], w=[ps])
        rstd = tmp
        self.act(rstd[:, 0, :], ps[:, 0:n], AF.Sqrt, r=[ps], w=[rstd], bias=1e-6, scale=1.0 / dim)
        self.S.op("vector", lambda e: e.reciprocal(out=rstd[:, 0, :], in_=rstd[:, 0, :]), r=[rstd], w=[rstd])
        for k in range(nk):
            self.stt(dst[:, k, :], src[:, k, :], g_ap[:, k:k + 1], rstd[:, 0, :], ALU.mult, ALU.mult, r=[src, g_t, rstd], w=[dst])

    def merge(self, l, bi, srcT, w_p, first):
        hT, acc = self.hT, self.acc
        gs = self.av("gs", [128, TT])
        tmp = self.av("mtmp", [128, TT])
        wg = self.dw["w_g"][l]
        for oc in range(8):
            py, pg = self.ps[oc % 2], self.ps[2 + oc % 2]
            sl = self.wslab(w_p[l], oc * 128, 128)
            for c in range(4):
                self.mm(py[:, :], sl[:, c, :], srcT[:, c, :], c == 0, c == 3, r=[sl, srcT], w=[py])
            self.proj(pg, 128, wg, bi * D + oc * 128, hT, pg)
            self.act(gs[:, :], pg[:, :], AF.Sigmoid, r=[pg], w=[gs])
            if first:
                self.tt("vector", acc[:, oc, :], py[:, :], gs[:, :], ALU.mult, r=[py, gs], w=[acc])
            else:
                self.tt("vector", tmp[:, :], py[:, :], gs[:, :], ALU.mult, r=[py, gs], w=[tmp])
                self.tt("gpsimd", acc[:, oc, :], acc[:, oc, :], tmp[:, :], ALU.add, r=[acc, tmp], w=[acc])

    def dump(self, name, tl, ap, dram_ap):
        if name in self.dbg_out:
            self.S.dma("sync", dram_ap(self.dbg_out[name]), ap, r=[tl], w=[self.dbg_out[name]])

    def build(self):
        S = self.S
        S.dma("sync", self.cst[:, :], self.d_cst, w=[self.cst])
        S.dma("sync", self.gfin[:, :], self.d_gfin, w=[self.gfin])
        for l in range(self.L):
            self.layer_setup(l)
            for ti in range(self.ntiles):
                self.tile(l, ti)
        S.emit(["outT"] + list(self.dbg_out.keys()))
        return self.nc

    def layer_setup(self, l):
        S = self.S
        self.phase(0)
        sm = self.small
        S.dma("sync", sm[:, :], self.d_small[l], w=[sm])
        self.memset("vector", self.gS[:, :, :], 0.0, w=[self.gS])
        self.memset("vector", self.gtail[:, :, :], 0.0, w=[self.gtail])
        self.memset("vector", self.ftail[:, :, :], 0.0, w=[self.ftail])
        self.memset("vector", self.s5c[:, :, :], 0.0, w=[self.s5c])
        self.act(self.nA[:, :], sm[:, SM_ALOG:SM_ALOG + 4], AF.Exp, r=[sm], w=[self.nA])
        self.ts("vector", self.nA[:, :], self.nA[:, :], -1.0, None, ALU.mult, None, r=[self.nA], w=[self.nA])
        if "b" in self.branches:
            self.s5_setup(l)

    def tile(self, l, ti):
        S = self.S
        t0 = ti * TT
        last = (l == self.L - 1)
        src = self.xT if l == 0 else self.x1T
        self.phase(0)
        self.hT = self.av("hT", [128, 8, TT])
        self.acc = self.av("acc", [128, 8, TT])
        self.smtok = self.av("smtok", [128, 4, 12])
        base = self.aoff
        xt = self.av("xt", [128, 8, TT])
        tmp = self.av("ntmp", [128, 8, TT])
        S.dma("gpsimd", xt[:, :, :], src.ap.rearrange("(k p) t -> p k t", p=128)[:, :, t0:t0 + TT], r=[src], w=[xt])
        self.rmsnorm(xt, self.small[:, SM_GA:SM_GA + 8], self.small, self.hT, tmp)
        wsm = self.wslab(self.dw["w_sm"][l], 0, 12)
        for qb in range(4):
            ps = self.ps[1]
            for k in range(8):
                self.mm(ps[:, 0:12], self.hT[:, k, qb * 128:(qb + 1) * 128], wsm[:, k, :], k == 0, k == 7, r=[self.hT, wsm], w=[ps])
            self.cp("vector", self.smtok[:, qb, :], ps[:, 0:12], r=[ps], w=[self.smtok])
        first = True
        for br in ("a", "b", "c"):
            if br not in self.branches:
                continue
            self.phase(base)
            if br == "a":
                oT = self.dsa(l, ti)
                self.merge(l, 0, oT, self.dw["w_pa"], first)
            elif br == "b":
                oT = self.s5(l, ti)
                self.merge(l, 1, oT, self.dw["w_pb"], first)
            else:
                oT = self.gdn(l, ti)
                self.merge(l, 2, oT, self.dw["w_pc"], first)
            first = False
        self.phase(base)
        xt = self.av("xt2", [128, 8, TT])
        S.dma("gpsimd", xt[:, :, :], src.ap.rearrange("(k p) t -> p k t", p=128)[:, :, t0:t0 + TT], r=[src], w=[xt])
        if first:
            pass
        else:
            for oc in range(8):
                ps = self.ps[oc % 2]
                self.proj(ps, 128, self.dw["w_out"][l], oc * 128, self.acc, ps)
                self.tt("vector", xt[:, oc, :], xt[:, oc, :], ps[:, :], ALU.add, r=[xt, ps], w=[xt])
        self.dump(f"xmid{l}", xt, xt[:, :, :], lambda d: d.ap.rearrange("(k p) t -> p k t", p=128)[:, :, t0:t0 + TT])
        if self.ffn:
            self.ffn_block(l, ti, xt)
        if last:
            self.rmsnorm(xt, self.gfin[:, :], self.gfin, xt, self.acc)
            dst = self.outT
        else:
            dst = self.x1T
        S.dma("gpsimd", dst.ap.rearrange("(k p) t -> p k t", p=128)[:, :, t0:t0 + TT], xt[:, :, :], r=[xt], w=[dst])

    def ffn_block(self, l, ti, xt):
        hT = self.hT
        self.rmsnorm(xt, self.small[:, SM_GF:SM_GF + 8], self.small, hT, self.acc)
        actT = self.av("actT", [128, 22, TT])
        ge = self.av("f_ge", [128, TT + 2])
        ve = self.av("f_ve", [128, TT + 2])
        gc = self.av("f_gc", [128, TT])
        vc = self.av("f_vc", [128, TT])
        sm, ft = self.small, self.ftail
        wup = self.dw["w_up"][l]

        def conv3(ext, j, out):
            cw = lambda i: sm[:, SM_FCW + j * 3 + i:SM_FCW + j * 3 + i + 1]
            self.ts("vector", out[:, :], ext[:, 0:TT], cw(0), None, ALU.mult, None, r=[ext, sm], w=[out])
            self.stt(out[:, :], ext[:, 1:TT + 1], cw(1), out[:, :], ALU.mult, ALU.add, r=[ext, sm, out], w=[out])
            self.stt(out[:, :], ext[:, 2:TT + 2], cw(2), out[:, :], ALU.mult, ALU.add, r=[ext, sm, out], w=[out])

        for j in range(22):
            pg, pv = self.ps[(2 * j) % 4], self.ps[(2 * j + 1) % 4]
            self.proj(pg, 128, wup, j * 128, hT, pg)
            self.proj(pv, 128, wup, DFF + j * 128, hT, pv)
            for (ps, ext, jj) in ((pg, ge, j), (pv, ve, 22 + j)):
                self.cp("gpsimd", ext[:, 0:2], ft[:, jj, :], r=[ft], w=[ext])
                self.cp("scalar", ext[:, 2:TT + 2], ps[:, :], r=[ps], w=[ext])
                self.cp("gpsimd", ft[:, jj, :], ext[:, TT:TT + 2], r=[ext], w=[ft])
            conv3(ge, j, gc)
            conv3(ve, 22 + j, vc)
            self.act(gc[:, :], gc[:, :], AF.Silu, r=[gc], w=[gc])
            self.tt("gpsimd", actT[:, j, :], gc[:, :], vc[:, :], ALU.mult, r=[gc, vc], w=[actT])
        wd = self.dw["w_down"][l]
        for oc in range(8):
            ps = self.ps[4 + oc % 2]
            s1 = self.wslab(wd, oc * 128, 128, nk=11, k0=0)
            s2 = self.wslab(wd, oc * 128, 128, nk=11, k0=11)
            for k in range(22):
                sl = s1 if k < 11 else s2
                self.mm(ps[:, :], sl[:, k % 11, :], actT[:, k, :], k == 0, k == 21, r=[sl, actT], w=[ps])
            self.tt("vector", xt[:, oc, :], xt[:, oc, :], ps[:, :], ALU.add, r=[xt, ps], w=[xt])

    def dsa(self, l, ti):
        raise NotImplementedError

    def s5_setup(self, l):
        raise NotImplementedError

    def s5(self, l, ti):
        raise NotImplementedError

    def gdn(self, l, ti):
        raise NotImplementedError


ARENA = 27300


def host_weights(p):
    w_in = p["w_in"]
    sl = lambda k: np.ascontiguousarray(w_in[:, :, W_SPLITS[k][0]:W_SPLITS[k][1]])
    w = {}
    w["w_q"] = sl("q")
    w["w_ckv"] = sl("ckv")
    w["w_iq"] = sl("iq")
    ik = sl("ik")
    w["w_ik2"] = np.ascontiguousarray(np.concatenate([ik, ik], axis=2))
    w["w_sm"] = np.ascontiguousarray(np.concatenate([sl("iw"), sl("a"), sl("b")], axis=2))
    w["w_u"] = sl("u")
    w["w_qkv"] = sl("qkv")
    w["w_z"] = sl("z")
    w["w_g"] = sl("gates")
    L = w_in.shape[0]
    wuk = p["w_uk"].reshape(L, 128, 4, 2, 64)
    w["wuk2"] = np.ascontiguousarray(wuk.transpose(0, 3, 4, 2, 1).reshape(L, 128, 512))
    wuvp = np.zeros((L, 128, 8, 128), np.float32)
    for h in range(8):
        wuvp[:, :, h, (h % 2) * 64:(h % 2) * 64 + 64] = p["w_uv"][:, :, h, :]
    w["wuvp"] = wuvp.reshape(L, 128, 1024)
    w["w_pa"] = p["w_proj_a"]
    w["w_pb"] = p["w_proj_b"]
    w["w_pc"] = p["w_proj_c"]
    w["w_glu"] = p["w_glu"]
    w["w_out"] = p["w_out"]
    w["w_up"] = p["w_up"]
    w["w_down"] = p["w_down"]
    return {k: np.ascontiguousarray(v, dtype=np.float32) for k, v in w.items()}


def host_common(p):
    m = host_weights(p)
    m["cst"] = make_consts()
    m["small"] = np.stack([pack_small(p, l) for l in range(2)])
    m["gfin"] = np.ascontiguousarray(p["final_norm_g"].reshape(8, 128).T)
    return m


_CACHE = {}


def kernel(**inputs):
    p = {k: np.asarray(v) for k, v in inputs.items()}
    x = p["x"]
    B = x.shape[0]
    common = host_common(p)
    if "nc" not in _CACHE:
        _CACHE["nc"] = Builder().build()
    nc = _CACHE["nc"]
    in_maps = []
    for b in range(B):
        m = dict(common)
        m["xT"] = np.ascontiguousarray(x[b].T)
        in_maps.append(m)
    res = run_bass_kernel_spmd(nc, in_maps, core_ids=list(range(B)))
    out = np.stack([np.ascontiguousarray(res.results[b]["outT"].T) for b in range(B)])
    return out.astype(np.float32)


TWO_PI = float(2 * np.pi)
MAGIC = 12582912.0


def _sin_of(B, out, arg, shift, t1, r_list, w_list):
    B.ts("vector", t1, arg, shift, 1.0 / TWO_PI, ALU.add, ALU.mult, r=r_list, w=w_list)
    B.ts("vector", t1, t1, MAGIC, MAGIC, ALU.add, ALU.subtract, r=w_list, w=w_list)
    B.stt(t1, t1, -TWO_PI, arg, ALU.mult, ALU.add, r=r_list + w_list, w=w_list)
    B.act(out, t1, AF.Sin, r=w_list, w=w_list, bias=float(shift), scale=1.0)


def s5_setup(self, l):
    sm, k = self.small, self.s5k
    are, aim, ldt = sm[:, SM_ARE:SM_ARE + 16], sm[:, SM_AIM:SM_AIM + 16], sm[:, SM_LDT:SM_LDT + 16]
    W = self.av("s5w", [128, 16, 16])
    w = lambda i: W[:, i, :]
    R, Wt = [sm, k, W], [W]
    self.act(w(0), ldt, AF.Exp, r=[sm], w=[W])
    self.tt("vector", w(1), are, w(0), ALU.mult, r=R, w=Wt)
    self.act(k[:, 0, :], w(1), AF.Exp, r=[W], w=[k])
    self.tt("vector", w(2), aim, w(0), ALU.mult, r=R, w=Wt)
    self.ts("vector", w(3), w(2), 1.0 / TWO_PI, None, ALU.mult, None, r=R, w=Wt)
    self.ts("vector", w(3), w(3), MAGIC, MAGIC, ALU.add, ALU.subtract, r=R, w=Wt)
    self.stt(k[:, 1, :], w(3), -TWO_PI, w(2), ALU.mult, ALU.add, r=R, w=[k])
    angr = k[:, 1, :]
    self.act(w(4), angr, AF.Sin, r=[k], w=[W])
    _sin_of(self, w(5), angr, float(np.pi / 2), w(6), [k, W], [W])
    self.tt("vector", w(6), k[:, 0, :], w(5), ALU.mult, r=R, w=Wt)
    self.tt("vector", w(7), k[:, 0, :], w(4), ALU.mult, r=R, w=Wt)
    self.ts("vector", w(6), w(6), -1.0, None, ALU.add, None, r=R, w=Wt)
    self.tt("vector", w(8), are, are, ALU.mult, r=R, w=Wt)
    self.tt("vector", w(9), aim, aim, ALU.mult, r=R, w=Wt)
    self.tt("vector", w(8), w(8), w(9), ALU.add, r=R, w=Wt)
    self.S.op("vector", lambda e: e.reciprocal(out=w(8), in_=w(8)), r=R, w=Wt)
    self.tt("vector", w(9), w(6), are, ALU.mult, r=R, w=Wt)
    self.tt("vector", w(10), w(7), aim, ALU.mult, r=R, w=Wt)
    self.tt("vector", w(9), w(9), w(10), ALU.add, r=R, w=Wt)
    self.tt("vector", w(9), w(9), w(8), ALU.mult, r=R, w=Wt)
    self.tt("vector", w(10), w(7), are, ALU.mult, r=R, w=Wt)
    self.tt("vector", w(11), w(6), aim, ALU.mult, r=R, w=Wt)
    self.tt("vector", w(10), w(10), w(11), ALU.subtract, r=R, w=Wt)
    self.tt("vector", w(10), w(10), w(8), ALU.mult, r=R, w=Wt)
    self.ts("vector", w(11), w(10), -1.0, None, ALU.mult, None, r=R, w=Wt)
    self.ts("vector", w(12), angr, 128.0, None, ALU.mult, None, r=R, w=Wt)
    _sin_of(self, k[:, 3, :], w(12), 0.0, w(13), [W, k], [k, W])
    _sin_of(self, k[:, 2, :], w(12), float(np.pi / 2), w(13), [W, k], [k, W])
    bb = self.av("s5bb", [128, 2, 16, 16])
    bre = sm[:, SM_BRE:SM_BRE + 256].rearrange("p (j m) -> p j m", j=16)
    bim = sm[:, SM_BIM:SM_BIM + 256].rearrange("p (j m) -> p j m", j=16)
    for j in range(16):
        fre, fim, nfim = w(9)[:, j:j + 1], w(10)[:, j:j + 1], w(11)[:, j:j + 1]
        self.ts("vector", bb[:, 0, j, :], bre[:, j, :], fre, None, ALU.mult, None, r=[sm, W], w=[bb])
        self.stt(bb[:, 0, j, :], bim[:, j, :], nfim, bb[:, 0, j, :], ALU.mult, ALU.add, r=[sm, W, bb], w=[bb])
        self.ts("vector", bb[:, 1, j, :], bim[:, j, :], fre, None, ALU.mult, None, r=[sm, W], w=[bb])
        self.stt(bb[:, 1, j, :], bre[:, j, :], fim, bb[:, 1, j, :], ALU.mult, ALU.add, r=[sm, W, bb], w=[bb])
    Zb = self.av("s5zb", [128, 2, 16, 32])
    self.memset("vector", Zb[:, :, :, :], 0.0, w=[Zb])
    self.memset("vector", self.Cz[:, :, :, :], 0.0, w=[self.Cz])
    cre = sm[:, SM_CRE:SM_CRE + 256].rearrange("p (j m) -> p j m", j=16)
    cim = sm[:, SM_CIM:SM_CIM + 256].rearrange("p (j m) -> p j m", j=16)
    for two in range(2):
        ps_, cs_ = slice(64 * two, 64 * two + 64), slice(16 * two, 16 * two + 16)
        for ri in range(2):
            self.cp("vector", Zb[ps_, ri, :, cs_], bb[ps_, ri, :, :], r=[bb], w=[Zb])
        self.cp("vector", self.Cz[ps_, 0, :, cs_], cre[ps_, :, :], r=[sm], w=[self.Cz])
        self.ts("vector", self.Cz[ps_, 1, :, cs_], cim[ps_, :, :], -1.0, None, ALU.mult, None, r=[sm], w=[self.Cz])
    for ri in range(2):
        for jq in range(4):
            ps = self.ps[(ri * 4 + jq) % 2]
            self.tr(ps[:, 0:128], Zb[:, ri, 4 * jq:4 * jq + 4, :], r=[Zb], w=[ps])
            self.cp("vector", self.Bc[:, ri, jq, :], ps[:, 0:128], r=[ps], w=[self.Bc])


def s5(self, l, ti):
    sm, k, cst = self.small, self.s5k, self.cst
    hT = self.hT
    uT = self.av("s5u", [128, 4, TT])
    tabS = self.av("s5ts", [128, 16, 128])
    tabC = self.av("s5tc", [128, 16, 128])
    ta = self.av("s5ta", [128, 16, 128])
    tb = self.av("s5tb", [128, 16, 128])
    yT = self.av("s5y", [128, 4, TT])
    sml = self.av("s5sm", [128, 8, 4])
    u4 = self.av("s5u4", [128, 4, 128])
    rrep = self.av("s5rr", [128, 16, 128])
    for j in range(16):
        self.cp("gpsimd", rrep[:, j, :], k[:, 0, j:j + 1].to_broadcast([128, 128]), r=[k], w=[rrep])
    for c in range(4):
        ps = self.ps[c % 2]
        self.proj(ps, 128, self.dw["w_u"][l], c * 128, hT, ps)
        self.cp("scalar", uT[:, c, :], ps[:, :], r=[ps], w=[uT])
    tau = cst[:, C_TAU:C_TAU + 128]
    for j in range(16):
        self.ts("vector", ta[:, j, :], tau, k[:, 1, j:j + 1], None, ALU.mult, None, r=[cst, k], w=[ta])
    _sin_of(self, tabS[:, :, :], ta[:, :, :], 0.0, tb[:, :, :], [ta], [tb, tabS])
    _sin_of(self, tabC[:, :, :], ta[:, :, :], float(np.pi / 2), tb[:, :, :], [ta], [tb, tabC])
    wk = lambda t, i: t[:, 4 * i:4 * i + 4, :]
    t1, t2, xr, xi = wk(ta, 0), wk(ta, 1), wk(ta, 2), wk(ta, 3)
    gre, gim, hr, hi = wk(tb, 0), wk(tb, 1), wk(tb, 2), wk(tb, 3)
    XR, XI, Y = self.ps[2], self.ps[3], self.ps[4]
    XRv = XR[:, :].rearrange("p (a b) -> p a b", a=4)
    XIv = XI[:, :].rearrange("p (a b) -> p a b", a=4)
    car = self.s5c
    for sc in range(4):
        c0 = sc * 128
        for jg in range(4):
            js = slice(4 * jg, 4 * jg + 4)
            for i in range(4):
                self.S.op("scalar", lambda e, i=i, jg=jg, c0=c0: e.mul(out=u4[:, i, :], in_=uT[:, jg, c0:c0 + 128], mul=cst[:, C_RM + i:C_RM + i + 1]),
                          r=[uT, cst], w=[u4])
            for i in range(4):
                for (P_, ri) in ((XR, 0), (XI, 1)):
                    self.mm(P_[:, i * 128:(i + 1) * 128], self.Bc[:, ri, jg, :], u4[:, i, :], True, True, r=[self.Bc, u4], w=[P_])
            cs_, sn_ = tabC[:, js, :], tabS[:, js, :]
            self.tt("vector", t1, XRv, cs_, ALU.mult, r=[XR, tabC], w=[ta])
            self.tt("vector", t2, XIv, sn_, ALU.mult, r=[XI, tabS], w=[ta])
            self.tt("gpsimd", xr, t1, t2, ALU.add, r=[ta], w=[ta])
            self.tt("vector", t1, XIv, cs_, ALU.mult, r=[XI, tabC, ta], w=[ta])
            self.tt("vector", t2, XRv, sn_, ALU.mult, r=[XR, tabS, ta], w=[ta])
            self.tt("gpsimd", xi, t1, t2, ALU.subtract, r=[ta], w=[ta])
            for i in range(4):
                j = 4 * jg + i
                rb = rrep[:, j, :]
                for (g_, x_, ci) in ((gre, xr, 0), (gim, xi, 1)):
                    self.S.op("vector", lambda e, g_=g_, x_=x_, ci=ci, i=i, j=j, rb=rb: e.tensor_tensor_scan(
                        out=g_[:, i, :], data0=rb, data1=x_[:, i, :], initial=car[:, ci, j:j + 1], op0=ALU.mult, op1=ALU.add),
                        r=[rrep, ta, car], w=[tb])
            lre, lim = gre[:, :, 127], gim[:, :, 127]
            c128, s128 = k[:, 2, js], k[:, 3, js]
            s_ = lambda i: sml[:, i, :]
            self.tt("vector", s_(0), c128, lre, ALU.mult, r=[k, tb], w=[sml])
            self.tt("vector", s_(1), s128, lim, ALU.mult, r=[k, tb], w=[sml])
            self.tt("vector", s_(2), s128, lre, ALU.mult, r=[k, tb], w=[sml])
            self.tt("vector", s_(3), c128, lim, ALU.mult, r=[k, tb], w=[sml])
            self.tt("vector", car[:, 0, js], s_(0), s_(1), ALU.subtract, r=[sml], w=[car])
            self.tt("vector", car[:, 1, js], s_(2), s_(3), ALU.add, r=[sml], w=[car])
            self.tt("gpsimd", hr, gre, cs_, ALU.mult, r=[tb, tabC], w=[tb])
            self.tt("gpsimd", t1, gim, sn_, ALU.mult, r=[tb, tabS, ta], w=[ta])
            self.tt("gpsimd", hr, hr, t1, ALU.subtract, r=[tb, ta], w=[tb])
            self.tt("gpsimd", hi, gre, sn_, ALU.mult, r=[tb, tabS], w=[tb])
            self.tt("gpsimd", t2, gim, cs_, ALU.mult, r=[tb, tabC, ta], w=[ta])
            self.tt("gpsimd", hi, hi, t2, ALU.add, r=[tb, ta], w=[tb])
            for i in range(4):
                j = 4 * jg + i
                rows = slice(32 * i, 32 * i + 32)
                self.mm(Y[rows, 0:128], self.Cz[:, 0, j, :], hr[:, i, :], True, False, r=[self.Cz, tb], w=[Y], tile_position=(0, 32 * i))
                self.mm(Y[rows, 0:128], self.Cz[:, 1, j, :], hi[:, i, :], False, True, r=[self.Cz, tb], w=[Y], tile_position=(0, 32 * i))
            self.stt(yT[:, jg, c0:c0 + 128], uT[:, jg, c0:c0 + 128], sm[:, SM_S5D + jg:SM_S5D + jg + 1], Y[:, 0:128], ALU.mult, ALU.add,
                     r=[uT, sm, Y], w=[yT])
    for c in range(4):
        self.act(yT[:, c, :], yT[:, c, :], AF.Gelu_apprx_tanh, r=[yT], w=[yT])
    wg = self.wslab(self.dw["w_glu"][l], 0, 512)
    o = uT
    for oc in range(4):
        ps = self.ps[oc % 2]
        for c in range(4):
            self.mm(ps[:, :], wg[:, c, oc * 128:(oc + 1) * 128], yT[:, c, :], c == 0, c == 3, r=[wg, yT], w=[ps])
        self.act(ta[:, 0:4, :], ps[:, :].rearrange("p (a b) -> p a b", a=4), AF.Sigmoid, r=[ps], w=[ta])
        self.tt("vector", o[:, oc, :], yT[:, oc, :], ta[:, 0:4, :].rearrange("p a b -> p (a b)"), ALU.mult, r=[yT, ta], w=[o])
    t0 = ti * TT
    self.dump("dbg_b", o, o[:, :, :], lambda d: d.ap.rearrange("(k p) t -> p k t", p=128)[:, :, t0:t0 + TT])
    return o


Builder.s5_setup = s5_setup
Builder.s5 = s5


def gdn(self, l, ti):
    sm, cst, hT = self.small, self.cst, self.hT
    ident = cst[:, C_ID:C_ID + 128]
    ones = cst[:, C_ONE:C_ONE + 128]
    qkv = self.av("g_qkv", [128, 12, TT])
    ext = self.av("g_ext", [128, TT + 3])
    oT = self.av("g_oT", [128, 4, TT])
    names = ["gbc", "egbc", "E1", "E2", "Dl", "Pa", "Pb", "PTa", "PTb", "TT", "kbg", "kdec", "vb", "nwT", "vn", "attnT", "qd", "bch"]
    W = {n: self.av("g_" + n, [128, 4, 128]) for n in names}
    sc = self.av("g_sc", [128, 12, 4])
    s_ = lambda i: sc[:, i, :]
    beta, gg, gamc, bg, kd, egc, t4 = s_(0), s_(1), s_(2), s_(3), s_(4), s_(5), s_(6)
    wq = self.dw["w_qkv"][l]
    for c in range(12):
        ps = self.ps[c % 2]
        self.proj(ps, 128, wq, c * 128, hT, ps)
        self.cp("gpsimd", ext[:, 0:3], self.gtail[:, c, :], r=[self.gtail], w=[ext])
        self.cp("scalar", ext[:, 3:TT + 3], ps[:, :], r=[ps], w=[ext])
        self.cp("gpsimd", self.gtail[:, c, :], ext[:, TT:TT + 3], r=[ext], w=[self.gtail])
        cw = lambda i: sm[:, SM_GCW + c * 4 + i:SM_GCW + c * 4 + i + 1]
        self.ts("vector", qkv[:, c, :], ext[:, 0:TT], cw(0), None, ALU.mult, None, r=[ext, sm], w=[qkv])
        for i in range(1, 4):
            self.stt(qkv[:, c, :], ext[:, i:TT + i], cw(i), qkv[:, c, :], ALU.mult, ALU.add, r=[ext, sm, qkv], w=[qkv])
        self.act(qkv[:, c, :], qkv[:, c, :], AF.Silu, r=[qkv], w=[qkv])
    import os as _os
    STOP = _os.environ.get("K_STOP", "")
    self.memset("vector", oT[:, :, :], 0.0, w=[oT])
    if STOP == "A":
        return oT
    tmp = W["Dl"]
    tmpf = tmp[:, :, :].rearrange("p a b -> p (a b)")
    for c in range(8):
        ps = self.ps[2 + c % 2]
        self.act(tmpf, qkv[:, c, :], AF.Square, r=[qkv], w=[tmp])
        self.mm(ps[:, :], ones, tmpf, True, True, r=[cst, tmp], w=[ps])
        self.act(tmpf, ps[:, :], AF.Sqrt, r=[ps], w=[tmp], bias=1e-6, scale=1.0)
        self.S.op("vector", lambda e: e.reciprocal(out=tmpf, in_=tmpf), r=[tmp], w=[tmp])
        if c < 4:
            self.stt(qkv[:, c, :], qkv[:, c, :], float(128 ** -0.5), tmpf, ALU.mult, ALU.mult, r=[qkv, tmp], w=[qkv])
        else:
            self.tt("vector", qkv[:, c, :], qkv[:, c, :], tmpf, ALU.mult, r=[qkv, tmp], w=[qkv])
    pA, pB, pC, pD = self.ps[4], self.ps[5], self.ps[6], self.ps[7]
    v4 = lambda p: p[:, :].rearrange("p (a b) -> p a b", a=4)
    if STOP == "B":
        return oT
    for blk in range(4):
        cs = slice(blk * 128, (blk + 1) * 128)
        qT = lambda h: qkv[:, h, cs]
        kT = lambda h: qkv[:, 4 + h, cs]
        vT = lambda h: qkv[:, 8 + h, cs]
        self.act(beta, self.smtok[:, blk, 8:12], AF.Sigmoid, r=[self.smtok], w=[sc])
        self.tt("vector", t4, self.smtok[:, blk, 4:8], sm[:, SM_DTB:SM_DTB + 4], ALU.add, r=[self.smtok, sm], w=[sc])
        self.act(t4, t4, AF.Exp, r=[sc], w=[sc])
        self.act(t4, t4, AF.Ln, r=[sc], w=[sc], bias=1.0, scale=1.0)
        self.tt("vector", gg, t4, self.nA[:, :], ALU.mult, r=[sc, self.nA], w=[sc])
        self.mm(pD[:, 0:4], cst[:, C_TRIU:C_TRIU + 128], gg, True, True, r=[cst, sc], w=[pD])
        self.cp("vector", gamc, pD[:, 0:4], r=[pD], w=[sc])
        gbc, egbc, E1, E2, Dl, bch = W["gbc"], W["egbc"], W["E1"], W["E2"], W["Dl"], W["bch"]
        for h in range(4):
            self.cp("vector", bch[:, h, :], gamc[:, h:h + 1].to_broadcast([128, 128]), r=[sc], w=[bch])
            self.mm(pA[:, h * 128:(h + 1) * 128], bch[:, h, :], ident, True, True, r=[bch, cst], w=[pA])
        self.cp("vector", gbc[:, :, :], v4(pA), r=[pA], w=[gbc])
        self.act(egbc[:, :, :], gbc[:, :, :], AF.Exp, r=[gbc], w=[egbc])
        for h in range(4):
            self.ts("vector", Dl[:, h, :], gbc[:, h, :], gamc[:, h:h + 1], None, ALU.subtract, None, r=[gbc, sc], w=[Dl])
        self.ts("vector", E1[:, :, :], Dl[:, :, :], 0.0, None, ALU.min, None, r=[Dl], w=[E1])
        self.act(E1[:, :, :], E1[:, :, :], AF.Exp, r=[E1], w=[E1])
        self.ts("vector", E2[:, :, :], Dl[:, :, :], 0.0, None, ALU.max, None, r=[Dl], w=[E2])
        self.act(E2[:, :, :], E2[:, :, :], AF.Exp, r=[E2], w=[E2], scale=-1.0)
        for h in range(4):
            self.tt("gpsimd", E1[:, h, :], E1[:, h, :], cst[:, C_MINC:C_MINC + 128], ALU.mult, r=[E1, cst], w=[E1])
            self.stt(E2[:, h, :], E2[:, h, :], -1.0, cst[:, C_MSTR:C_MSTR + 128], ALU.mult, ALU.mult, r=[E2, cst], w=[E2])
        self.act(egc, gamc, AF.Exp, r=[sc], w=[sc])
        self.tt("vector", bg, beta, egc, ALU.mult, r=[sc], w=[sc])
        self.tt("vector", kd, gbc[:, :, 127], gamc, ALU.subtract, r=[gbc, sc], w=[sc])
        self.act(kd, kd, AF.Exp, r=[sc], w=[sc])
        if STOP == "C":
            return oT
        Pa, Pb, PTa, PTb, TTt = W["Pa"], W["Pb"], W["PTa"], W["PTb"], W["TT"]
        for h in range(4):
            self.mm(pA[:, h * 128:(h + 1) * 128], kT(h), kT(h), True, True, r=[qkv], w=[pA])
        for h in range(4):
            self.stt(Pa[:, h, :], pA[:, h * 128:(h + 1) * 128], beta[:, h:h + 1], E2[:, h, :], ALU.mult, ALU.mult, r=[pA, sc, E2], w=[Pa])
        for h in range(4):
            self.tr(pB[:, h * 128:(h + 1) * 128], Pa[:, h, :], r=[Pa], w=[pB])
        self.cp("scalar", PTa[:, :, :], v4(pB), r=[pB], w=[PTa])
        for h in range(4):
            self.tt("vector", TTt[:, h, :], pB[:, h * 128:(h + 1) * 128], ident, ALU.add, r=[pB, cst], w=[TTt])
        P, PT, P2, PT2 = Pa, PTa, Pb, PTb
        for lev in range(1, 7):
            for h in range(4):
                hs = slice(h * 128, (h + 1) * 128)
                self.mm(pA[:, hs], PT[:, h, :], P[:, h, :], True, True, r=[PT, P], w=[pA])
            if lev < 6:
                for h in range(4):
                    hs = slice(h * 128, (h + 1) * 128)
                    self.mm(pB[:, hs], P[:, h, :], PT[:, h, :], True, True, r=[PT, P], w=[pB])
            self.cp("scalar", P2[:, :, :], v4(pA), r=[pA], w=[P2])
            if lev < 6:
                self.cp("vector", PT2[:, :, :], v4(pB), r=[pB], w=[PT2])
            for h in range(4):
                hs = slice(h * 128, (h + 1) * 128)
                self.mm(pC[:, hs], P2[:, h, :], TTt[:, h, :], True, True, r=[P2, TTt], w=[pC])
            self.tt("vector", TTt[:, :, :], TTt[:, :, :], v4(pC), ALU.add, r=[TTt, pC], w=[TTt])
            P, PT, P2, PT2 = P2, PT2, P, PT
        if STOP == "D":
            return oT
        attnT, kbg, kdec, vb, nwT, vn, qd = W["attnT"], W["kbg"], W["kdec"], W["vb"], W["nwT"], W["vn"], W["qd"]
        for h in range(4):
            self.mm(pA[:, h * 128:(h + 1) * 128], kT(h), qT(h), True, True, r=[qkv], w=[pA])
        self.tt("vector", attnT[:, :, :], v4(pA), E1[:, :, :], ALU.mult, r=[pA, E1], w=[attnT])
        for h in range(4):
            self.tr(pB[:, h * 128:(h + 1) * 128], kT(h), r=[qkv], w=[pB])
            self.tr(pC[:, h * 128:(h + 1) * 128], vT(h), r=[qkv], w=[pC])
        for h in range(4):
            hs = slice(h * 128, (h + 1) * 128)
            self.ts("vector", kbg[:, h, :], pB[:, hs], bg[:, h:h + 1], None, ALU.mult, None, r=[pB, sc], w=[kbg])
            self.ts("vector", kdec[:, h, :], pB[:, hs], kd[:, h:h + 1], None, ALU.mult, None, r=[pB, sc], w=[kdec])
            self.ts("vector", vb[:, h, :], pC[:, hs], beta[:, h:h + 1], None, ALU.mult, None, r=[pC, sc], w=[vb])
        for h in range(4):
            self.mm(pA[:, h * 128:(h + 1) * 128], kbg[:, h, :], TTt[:, h, :], True, True, r=[kbg, TTt], w=[pA])
        self.ts("vector", nwT[:, :, :], v4(pA), -1.0, None, ALU.mult, None, r=[pA], w=[nwT])
        gS = self.gS
        for h in range(4):
            hs = slice(h * 128, (h + 1) * 128)
            self.mm(pB[:, hs], TTt[:, h, :], vb[:, h, :], True, False, r=[TTt, vb], w=[pB])
            self.mm(pB[:, hs], nwT[:, h, :], gS[:, h, :], False, True, r=[nwT, gS], w=[pB])
        self.cp("scalar", vn[:, :, :], v4(pB), r=[pB], w=[vn])
        self.tt("gpsimd", qd[:, :, :], qkv[:, 0:4, cs], egbc[:, :, :], ALU.mult, r=[qkv, egbc], w=[qd])
        for h in range(4):
            hs = slice(h * 128, (h + 1) * 128)
            self.mm(pC[:, hs], gS[:, h, :], qd[:, h, :], True, False, r=[gS, qd], w=[pC])
            self.mm(pC[:, hs], vn[:, h, :], attnT[:, h, :], False, True, r=[vn, attnT], w=[pC])
        self.cp("scalar", oT[:, :, cs], v4(pC), r=[pC], w=[oT])
        for h in range(4):
            self.mm(pA[:, h * 128:(h + 1) * 128], kdec[:, h, :], vn[:, h, :], True, True, r=[kdec, vn], w=[pA])
        for h in range(4):
            self.stt(gS[:, h, :], gS[:, h, :], egbc[:, h, 127:128], pA[:, h * 128:(h + 1) * 128], ALU.mult, ALU.add, r=[gS, egbc, pA], w=[gS])
    if STOP == "E":
        return oT
    zs = W["bch"][:, :, :].rearrange("p a b -> p (a b)")
    for h in range(4):
        ps, pz = self.ps[2 + h % 2], self.ps[h % 2]
        self.act(tmpf, oT[:, h, :], AF.Square, r=[oT], w=[tmp])
        self.mm(ps[:, :], ones, tmpf, True, True, r=[cst, tmp], w=[ps])
        self.act(tmpf, ps[:, :], AF.Sqrt, r=[ps], w=[tmp], bias=1e-6, scale=1.0 / 128)
        self.S.op("vector", lambda e: e.reciprocal(out=tmpf, in_=tmpf), r=[tmp], w=[tmp])
        self.stt(oT[:, h, :], oT[:, h, :], sm[:, SM_GNG:SM_GNG + 1], tmpf, ALU.mult, ALU.mult, r=[oT, sm, tmp], w=[oT])
        self.proj(pz, 128, self.dw["w_z"][l], h * 128, hT, pz)
        self.act(zs, pz[:, :], AF.Silu, r=[pz], w=[W["bch"]])
        self.tt("vector", oT[:, h, :], oT[:, h, :], zs, ALU.mult, r=[oT, W["bch"]], w=[oT])
    t0 = ti * TT
    self.dump("dbg_c", oT, oT[:, :, :], lambda d: d.ap.rearrange("(k p) t -> p k t", p=128)[:, :, t0:t0 + TT])
    return oT


Builder.gdn = gdn


TOPK = 256


def dsa(self, l, ti):
    sm, cst, hT = self.small, self.cst, self.hT
    ident = cst[:, C_ID:C_ID + 128]
    ones = cst[:, C_ONE:C_ONE + 128]
    t0 = ti * TT
    qT = self.av("a_qT", [128, 4, TT])
    iqT = self.av("a_iqT", [128, 2, TT])
    oT = self.av("a_oT", [128, 4, TT])
    Ssc = self.av("a_S", [128, T])
    qlat = self.av("a_qlat", [128, 8, 128])
    iwbc = self.av("a_iwbc", [128, 4, 128])
    J = self.av("a_J", [128, T])
    def sub(name, off, a):
        return Tl(self.S, J.ap[:, off:off + a * 128].rearrange("p (a b) -> p a b", a=a), name)
    E, rr, bc4, olat, rd = sub("a_E", 0, 8), sub("a_rr", 1024, 4), sub("a_bc4", 1536, 4), sub("a_olat", 2048, 8), sub("a_rd", 3072, 8)
    jal = [E, rr, bc4, olat, rd]
    sT = self.av("a_sT", [128, 128])
    mT = self.av("a_mT", [128, 128])
    thrbc = self.av("a_thrbc", [128, 128])
    tmpn = self.av("a_tmpn", [128, TT])
    st = self.av("a_st", [128, 8])
    mx, mn, rng, lo, mid, cnt, ind, thr = [st[:, i:i + 1] for i in range(8)]
    v4 = lambda p: p[:, :].rearrange("p (a b) -> p a b", a=4)
    self.memset("vector", st[:, :], 0.0, w=[st])
    for c in range(4):
        ps = self.ps[c % 2]
        self.proj(ps, 128, self.dw["w_q"][l], c * 128, hT, ps)
        self.cp("scalar", qT[:, c, :], ps[:, :], r=[ps], w=[qT])
    for c in range(2):
        ps = self.ps[c % 2]
        self.proj(ps, 128, self.dw["w_iq"][l], c * 128, hT, ps)
        self.cp("scalar", iqT[:, c, :], ps[:, :], r=[ps], w=[iqT])
    ps = self.ps[0]
    self.proj(ps, 128, self.dw["w_ik2"][l], 0, hT, ps)
    self.cp("scalar", self.ikT[:, t0:t0 + TT], ps[:, :], r=[ps], w=[self.ikT])
    pc, pn = self.ps[1], self.ps[2]
    self.proj(pc, 128, self.dw["w_ckv"][l], 0, hT, pc)
    self.act(tmpn[:, :], pc[:, :], AF.Square, r=[pc], w=[tmpn])
    self.mm(pn[:, :], ones, tmpn[:, :], True, True, r=[cst, tmpn], w=[pn])
    self.act(tmpn[:, :], pn[:, :], AF.Sqrt, r=[pn], w=[tmpn], bias=1e-6, scale=1.0 / 128)
    self.S.op("vector", lambda e: e.reciprocal(out=tmpn[:, :], in_=tmpn[:, :]), r=[tmpn], w=[tmpn])
    self.stt(self.cT[:, t0:t0 + TT], pc[:, :], sm[:, SM_KVG:SM_KVG + 1], tmpn[:, :], ALU.mult, ALU.mult, r=[pc, sm, tmpn], w=[self.cT])
    for kb in range(4):
        ps = self.ps[kb % 2]
        self.tr(ps[:, 0:128], self.cT[:, t0 + kb * 128:t0 + (kb + 1) * 128], r=[self.cT], w=[ps])
        self.cp("vector", self.ctok[:, ti * 4 + kb, :], ps[:, 0:128], r=[ps], w=[self.ctok])
    wuk = self.slab(self.dw["wuk2"][l].rearrange("p (c r) -> p c r", c=4), [128, 4, 128])
    iw = lambda qb, h: self.smtok[:, qb, h:h + 1]
    for qb in range(4):
        qs = slice(qb * 128, (qb + 1) * 128)
        q0 = t0 + qb * 128
        Lk = q0 + 128
        nkc = Lk // 128
        pA, pB, pL0, pL1, pO0, pO1, pD0, pD1 = self.ps
        for h in range(8):
            rows = slice((h % 2) * 64, (h % 2) * 64 + 64)
            pp = pL0 if h % 2 == 0 else pL1
            self.mm(pp[:, (h // 2) * 128:(h // 2 + 1) * 128], wuk[rows, h // 2, :], qT[rows, h // 2, qs], True, True, r=[wuk, qT], w=[pp])
        self.S.op("scalar", lambda e: e.mul(out=qlat[:, 0:4, :], in_=v4(pL0), mul=0.125), r=[pL0], w=[qlat])
        self.S.op("scalar", lambda e: e.mul(out=qlat[:, 4:8, :], in_=v4(pL1), mul=0.125), r=[pL1], w=[qlat])
        for h in range(4):
            sl4 = (h % 2) * 2 + h // 2
            self.cp("vector", bc4[:, h, :], iw(qb, h).to_broadcast([128, 128]), r=[self.smtok], w=[bc4])
            self.mm(pA[:, sl4 * 128:(sl4 + 1) * 128], bc4[:, h, :], ident, True, True, r=[bc4, cst], w=[pA])
        self.cp("vector", iwbc[:, :, :], v4(pA), r=[pA], w=[iwbc])
        if Lk > TOPK:
            nseg = (Lk + 511) // 512
            for seg in range(nseg):
                wd = min(512, Lk - seg * 512)
                ks = slice(seg * 512, seg * 512 + wd)
                for h in range(4):
                    rows = slice((h % 2) * 64, (h % 2) * 64 + 64)
                    pp = pA if h % 2 == 0 else pB
                    self.mm(pp[:, 0:wd], iqT[rows, h // 2, qs], self.ikT[rows, ks], True, True, r=[iqT, self.ikT], w=[pp])
                    self.act(tmpn[:, 0:wd], pp[:, 0:wd], AF.Relu, r=[pp], w=[tmpn])
                    if h == 0:
                        self.ts("vector", Ssc[:, ks], tmpn[:, 0:wd], iw(qb, 0), None, ALU.mult, None, r=[tmpn, self.smtok], w=[Ssc])
                    else:
                        self.stt(Ssc[:, ks], tmpn[:, 0:wd], iw(qb, h), Ssc[:, ks], ALU.mult, ALU.add, r=[tmpn, self.smtok, Ssc], w=[Ssc])
            self.S.op("vector", lambda e, Lk=Lk: e.tensor_reduce(out=mx, in_=Ssc[:, 0:Lk], axis=AX.X, op=ALU.max), r=[Ssc], w=[st])
            self.S.op("vector", lambda e, Lk=Lk: e.tensor_reduce(out=mn, in_=Ssc[:, 0:Lk], axis=AX.X, op=ALU.min), r=[Ssc], w=[st])
            self.tt("vector", rng, mx, mn, ALU.subtract, r=[st], w=[st])
            self.cp("vector", lo, mn, r=[st], w=[st])
            self.memset("vector", Ssc[0:64, Lk - 64:Lk], NEG, w=[Ssc])
            if "dbg_S" in self.dbg_out and ti == 0 and qb == 2:
                self.S.dma("sync", self.dbg_out["dbg_S"].ap[0:128, 0:Lk], Ssc[:, 0:Lk], r=[Ssc], w=[self.dbg_out["dbg_S"]])
            for it in range(NIT):
                f = float(2.0 ** -(it + 1))
                self.stt(mid, rng, f, lo, ALU.mult, ALU.add, r=[st], w=[st])
                self.S.op("vector", lambda e, Lk=Lk: e.tensor_scalar(out=J.ap[:, 0:Lk], in0=Ssc[:, 0:Lk], scalar1=mid, scalar2=None,
                                                              op0=ALU.is_ge, op1=ALU.add, accum_out=cnt), r=[Ssc, st], w=jal + [st])
                self.ts("vector", ind, cnt, float(TOPK), f, ALU.is_ge, ALU.mult, r=[st], w=[st])
                self.stt(lo, ind, rng, lo, ALU.mult, ALU.add, r=[st], w=[st])
            self.cp("vector", thr, lo, r=[st], w=[st])
            if "dbg_S2" in self.dbg_out and ti == 0 and qb == 2:
                self.S.dma("sync", self.dbg_out["dbg_S2"].ap[0:128, 0:Lk], Ssc[:, 0:Lk], r=[Ssc], w=[self.dbg_out["dbg_S2"]])
                self.S.dma("sync", self.dbg_out["dbg_S2"].ap[128:256, 0:Lk], J.ap[:, 0:Lk], r=jal, w=[self.dbg_out["dbg_S2"]])
        else:
            self.memset("vector", thr, NEG, w=[st])
        if "dbg_thr" in self.dbg_out:
            self.S.dma("sync", self.dbg_out["dbg_thr"].ap[0:128, (ti * 4 + qb) * 8:(ti * 4 + qb) * 8 + 8], st[:, 0:8], r=[st], w=[self.dbg_out["dbg_thr"]])
        self.cp("vector", bc4[:, 0, :], thr.to_broadcast([128, 128]), r=[st], w=[bc4])
        self.mm(pA[:, 0:128], bc4[:, 0, :], ident, True, True, r=[bc4, cst], w=[pA])
        self.cp("vector", thrbc[:, :], pA[:, 0:128], r=[pA], w=[thrbc])
        for kc in range(nkc):
            ks = slice(kc * 128, (kc + 1) * 128)
            for h in range(4):
                rows = slice((h % 2) * 64, (h % 2) * 64 + 64)
                pp = pB if h % 2 == 0 else pA
                self.mm(pp[:, (h // 2) * 128:(h // 2 + 1) * 128], self.ikT[rows, ks], iqT[rows, h // 2, qs], True, True, r=[self.ikT, iqT], w=[pp])
            self.act(rr[:, 0:2, :], pB[:, 0:256].rearrange("p (a b) -> p a b", a=2), AF.Relu, r=[pB], w=[rr])
            self.act(rr[:, 2:4, :], pA[:, 0:256].rearrange("p (a b) -> p a b", a=2), AF.Relu, r=[pA], w=[rr])
            self.tt("vector", rr[:, :, :], rr[:, :, :], iwbc[:, :, :], ALU.mult, r=[rr, iwbc], w=[rr])
            self.tt("vector", sT[:, :], rr[:, 0, :], rr[:, 1, :], ALU.add, r=[rr], w=[sT])
            self.tt("vector", sT[:, :], sT[:, :], rr[:, 2, :], ALU.add, r=[rr, sT], w=[sT])
            self.tt("vector", sT[:, :], sT[:, :], rr[:, 3, :], ALU.add, r=[rr, sT], w=[sT])
            self.tt("vector", mT[:, :], sT[:, :], thrbc[:, :], ALU.is_ge, r=[sT, thrbc], w=[mT])
            if kc == nkc - 1:
                self.memset("vector", mT[64:128, 0:64], 0.0, w=[mT])
            self.mm(pL0[:, :], self.cT[:, ks], qlat[:, 0:4, :], True, True, r=[self.cT, qlat], w=[pL0])
            self.mm(pL1[:, :], self.cT[:, ks], qlat[:, 4:8, :], True, True, r=[self.cT, qlat], w=[pL1])
            self.act(E[:, 0:4, :], v4(pL0), AF.Exp, r=[pL0], w=[E])
            self.act(E[:, 4:8, :], v4(pL1), AF.Exp, r=[pL1], w=[E])
            for h in range(8):
                self.tt("gpsimd" if h % 2 else "vector", E[:, h, :], E[:, h, :], mT[:, :], ALU.mult, r=[E, mT], w=[E])
            st_, sp_ = (kc == 0), (kc == nkc - 1)
            self.mm(pO0[:, :], self.ctok[:, kc, :], E[:, 0:4, :], st_, sp_, r=[self.ctok, E], w=[pO0])
            self.mm(pO1[:, :], self.ctok[:, kc, :], E[:, 4:8, :], st_, sp_, r=[self.ctok, E], w=[pO1])
            self.mm(pD0[:, :], ones, E[:, 0:4, :], st_, sp_, r=[cst, E], w=[pD0])
            self.mm(pD1[:, :], ones, E[:, 4:8, :], st_, sp_, r=[cst, E], w=[pD1])
        self.S.op("vector", lambda e: e.reciprocal(out=rd[:, 0:4, :], in_=v4(pD0)), r=[pD0], w=[rd])
        self.S.op("vector", lambda e: e.reciprocal(out=rd[:, 4:8, :], in_=v4(pD1)), r=[pD1], w=[rd])
        self.tt("vector", olat[:, 0:4, :], v4(pO0), rd[:, 0:4, :], ALU.mult, r=[pO0, rd], w=[olat])
        self.tt("vector", olat[:, 4:8, :], v4(pO1), rd[:, 4:8, :], ALU.mult, r=[pO1, rd], w=[olat])
        wuv = self.slab(self.dw["wuvp"][l].rearrange("p (h c) -> p h c", h=8), [128, 8, 128])
        for c in range(4):
            self.mm(pA[:, c * 128:(c + 1) * 128], wuv[:, 2 * c, :], olat[:, c, :], True, False, r=[wuv, olat], w=[pA])
            self.mm(pA[:, c * 128:(c + 1) * 128], wuv[:, 2 * c + 1, :], olat[:, 4 + c, :], False, True, r=[wuv, olat], w=[pA])
        self.cp("scalar", oT[:, :, qs], v4(pA), r=[pA], w=[oT])
    self.dump("dbg_a", oT, oT[:, :, :], lambda d: d.ap.rearrange("(k p) t -> p k t", p=128)[:, :, t0:t0 + TT])
    return oT


Builder.dsa = dsa
```
